# Optimizing a Trainium2 kernel written in Bass

```python
import jax, jax.numpy as jnp
from jax import lax
import numpy as np

D_MODEL = 1024
BATCH = 2
SEQ = 8192
DEPTH = 2
DEC_BATCH = 128
DEC_SEQ = 1
PAST_LEN = 8192
PAGE_SIZE = 128

HEAD_DIM = 64
A_GROUPS = ((128, 1), (512, 4), (2048, 16))
N_GROUPS_A = len(A_GROUPS)
H_A = D_MODEL // HEAD_DIM
HQ_B = D_MODEL // HEAD_DIM
HKV_B = 2
WINDOW_B = 128
BLOCK = 128
D_FF_DENSE = 2816
N_EXPERTS = 8
TOP_K = 2
D_FF_EXPERT = 1408
RMS_EPS = 1e-6
N_EVEN_LAYERS = (DEPTH + 1) // 2
N_ODD_LAYERS = DEPTH // 2

kernel_name = 'hybrid_dilated_swa_sink_decoder_step'


def rms_norm(x, g):
    xf = x.astype(jnp.float32)
    y = xf * lax.rsqrt(jnp.mean(xf * xf, axis=-1, keepdims=True) + RMS_EPS)
    return (y * g.astype(jnp.float32)).astype(x.dtype)


def alibi_slopes(n_heads):
    return jnp.asarray(2.0 ** (-8.0 * np.arange(1, n_heads + 1) / n_heads), dtype=jnp.float32)


def softmax_with_sink(s, sink):
    m = jnp.max(s, axis=-1)
    if sink is not None:
        m = jnp.maximum(m, sink)
    p = jnp.exp(s - m[..., None])
    denom = jnp.sum(p, axis=-1)
    if sink is not None:
        denom = denom + jnp.exp(sink - m)
    return p / denom[..., None], m + jnp.log(denom)


def banded_window_attention(q, k, v, step, n_steps, slopes, sink):
    N, L, Hq, hd = q.shape
    Hk = k.shape[2]
    G = Hq // Hk
    nb = -(-L // BLOCK)
    pad = nb * BLOCK - L
    qb = jnp.pad(q, ((0, 0), (0, pad), (0, 0), (0, 0))).reshape(N, nb, BLOCK, Hk, G, hd)

    def key_windows(a):
        a = jnp.pad(a, ((0, 0), (BLOCK, pad), (0, 0), (0, 0))).reshape(N, nb + 1, BLOCK, Hk, hd)
        return jnp.concatenate([a[:, :-1], a[:, 1:]], axis=2)

    kw, vw = key_windows(k), key_windows(v)
    s = jnp.einsum('ncqkgd,ncskd->nckgqs', qb, kw, preferred_element_type=jnp.float32) * (hd ** -0.5)
    qi = jnp.arange(BLOCK)[:, None]
    sj = jnp.arange(2 * BLOCK)[None, :]
    dist = qi - sj + BLOCK
    in_band = (dist >= 0) & (dist <= n_steps)
    key_row = (jnp.arange(nb) * BLOCK - BLOCK)[:, None, None] + sj[None]
    mask = in_band[None] & (key_row >= 0)
    bias = -(slopes.reshape(Hk, G, 1, 1) * (step * dist).astype(jnp.float32))
    s = jnp.where(mask[None, :, None, None], s + bias, -jnp.inf)
    sink_r = None if sink is None else sink.astype(jnp.float32).reshape(Hk, G, 1)
    p, lse = softmax_with_sink(s, sink_r)
    o = jnp.einsum('nckgqs,ncskd->ncqkgd', p.astype(vw.dtype), vw).reshape(N, nb * BLOCK, Hq, hd)[:, :L]
    lse = lse.transpose(0, 1, 4, 2, 3).reshape(N, nb * BLOCK, Hq)[:, :L]
    return o, lse


def window_attention_step(q, kv_buf, kv_new, step, n_steps, slopes, sink):
    N, T, Hq, hd = q.shape
    L, Hk = kv_buf.shape[1], kv_buf.shape[3]
    G = Hq // Hk
    j = jnp.arange(n_steps + 1)
    idx = L + jnp.arange(T)[:, None] - step * j[None, :]
    valid = idx >= 0
    in_buf = (idx < L)[None, :, :, None, None, None]
    rows = jnp.where(in_buf, kv_buf[:, jnp.clip(idx, 0, L - 1)], kv_new[:, jnp.clip(idx - L, 0, T - 1)])
    kg, vg = rows[:, :, :, 0], rows[:, :, :, 1]
    s = jnp.einsum('ntkgd,ntjkd->ntkgj', q.reshape(N, T, Hk, G, hd), kg,
                   preferred_element_type=jnp.float32) * (hd ** -0.5)
    bias = -(slopes.reshape(Hk, G, 1) * (step * j).astype(jnp.float32))
    s = jnp.where(valid[None, :, None, None, :], s + bias, -jnp.inf)
    sink_r = None if sink is None else sink.astype(jnp.float32).reshape(Hk, G)
    p, lse = softmax_with_sink(s, sink_r)
    o = jnp.einsum('ntkgj,ntjkd->ntkgd', p.astype(vg.dtype), vg).reshape(N, T, Hq, hd)
    return o, lse.reshape(N, T, Hq)


def qkv_dilated(h, w_in, q_gain, k_gain):
    N, T, _ = h.shape
    qkv = jnp.einsum('ntd,dc->ntc', h, w_in).reshape(N, T, N_GROUPS_A, 3, H_A, HEAD_DIM)
    q = rms_norm(qkv[:, :, :, 0], q_gain[:, None, :])
    k = rms_norm(qkv[:, :, :, 1], k_gain[:, None, :])
    return q, k, qkv[:, :, :, 2]


def dilated_group_prompt(q, k, v, dil, n_steps, slopes):
    B, S, H, hd = q.shape
    Ls = S // dil

    def split(a):
        return a.reshape(B, Ls, dil, H, hd).transpose(0, 2, 1, 3, 4).reshape(B * dil, Ls, H, hd)

    o, lse = banded_window_attention(split(q), split(k), split(v), dil, n_steps, slopes, None)
    o = o.reshape(B, dil, Ls, H, hd).transpose(0, 2, 1, 3, 4).reshape(B, S, H, hd)
    lse = lse.reshape(B, dil, Ls, H).transpose(0, 2, 1, 3).reshape(B, S, H)
    return o, lse


def merge_by_denominator(outs, lses):
    w = jax.nn.softmax(jnp.stack(lses, axis=0), axis=0)
    o = jnp.stack(outs, axis=0)
    return jnp.sum(w[..., None].astype(o.dtype) * o, axis=0)


def qkv_swa(h, w_in, q_gain, k_gain):
    N, T, _ = h.shape
    qkv = jnp.einsum('ntd,dc->ntc', h, w_in)
    nq, nk = HQ_B * HEAD_DIM, HKV_B * HEAD_DIM
    q = qkv[..., :nq].reshape(N, T, HQ_B, HEAD_DIM)
    k = qkv[..., nq:nq + nk].reshape(N, T, HKV_B, HEAD_DIM)
    v = qkv[..., nq + nk:].reshape(N, T, HKV_B, HEAD_DIM)
    return rms_norm(q, q_gain), rms_norm(k, k_gain), v


def out_proj(o, w_out):
    N, T = o.shape[:2]
    return jnp.einsum('ntc,cd->ntd', o.reshape(N, T, -1), w_out)


def swiglu(h, w_gu, w_down):
    g, u = jnp.split(h @ w_gu, 2, axis=-1)
    return (jax.nn.silu(g) * u) @ w_down


def moe_swiglu(h, w_router, w_gu, w_down):
    N, T, D = h.shape
    hf = h.reshape(N * T, D)
    logits = jnp.einsum('md,de->me', hf, w_router, preferred_element_type=jnp.float32)
    top_logit, top_idx = lax.top_k(logits, TOP_K)
    top_gate = jax.nn.softmax(top_logit, axis=-1)
    gate = jnp.sum(jax.nn.one_hot(top_idx, N_EXPERTS, dtype=jnp.float32) * top_gate[..., None], axis=1)
    y = jnp.zeros_like(hf)
    for e in range(N_EXPERTS):
        y = y + gate[:, e:e + 1].astype(hf.dtype) * swiglu(hf, w_gu[e], w_down[e])
    return y.reshape(N, T, D)


def setup_inputs(seed: int = 0) -> dict:
    key = jax.random.key(seed)
    ks = iter(jax.random.split(key, 32))

    def nrm(shape, scale=1.0):
        return jax.random.normal(next(ks), shape, jnp.float32) * scale

    ne, no = N_EVEN_LAYERS, N_ODD_LAYERS
    cols_a = N_GROUPS_A * 3 * H_A * HEAD_DIM
    cols_b = (HQ_B + 2 * HKV_B) * HEAD_DIM
    return {
        'x_prompt': nrm((BATCH, SEQ, D_MODEL)),
        'x_sample': nrm((DEC_BATCH, DEC_SEQ, D_MODEL)),
        'cache_a_w128': nrm((ne, DEC_BATCH, min(A_GROUPS[0][0], PAST_LEN), 2, H_A, HEAD_DIM)),
        'cache_a_w512': nrm((ne, DEC_BATCH, min(A_GROUPS[1][0], PAST_LEN), 2, H_A, HEAD_DIM)),
        'cache_a_w2048': nrm((ne, DEC_BATCH, min(A_GROUPS[2][0], PAST_LEN), 2, H_A, HEAD_DIM)),
        'cache_b': nrm((no, DEC_BATCH, min(WINDOW_B, PAST_LEN), 2, HKV_B, HEAD_DIM)),
        'norm_mix_a': 1.0 + nrm((ne, D_MODEL), 0.02),
        'w_in_a': nrm((ne, D_MODEL, cols_a), D_MODEL ** -0.5),
        'q_gain_a': 1.0 + nrm((ne, N_GROUPS_A, HEAD_DIM), 0.02),
        'k_gain_a': 1.0 + nrm((ne, N_GROUPS_A, HEAD_DIM), 0.02),
        'w_out_a': nrm((ne, H_A * HEAD_DIM, D_MODEL), (H_A * HEAD_DIM) ** -0.5),
        'norm_ffn_dense': 1.0 + nrm((ne, D_MODEL), 0.02),
        'w_gu_dense': nrm((ne, D_MODEL, 2 * D_FF_DENSE), D_MODEL ** -0.5),
        'w_down_dense': nrm((ne, D_FF_DENSE, D_MODEL), D_FF_DENSE ** -0.5),
        'norm_mix_b': 1.0 + nrm((no, D_MODEL), 0.02),
        'w_in_b': nrm((no, D_MODEL, cols_b), D_MODEL ** -0.5),
        'q_gain_b': 1.0 + nrm((no, HEAD_DIM), 0.02),
        'k_gain_b': 1.0 + nrm((no, HEAD_DIM), 0.02),
        'sink_b': nrm((no, HQ_B), 0.5),
        'w_out_b': nrm((no, HQ_B * HEAD_DIM, D_MODEL), (HQ_B * HEAD_DIM) ** -0.5),
        'norm_ffn_moe': 1.0 + nrm((no, D_MODEL), 0.02),
        'w_router': nrm((no, D_MODEL, N_EXPERTS), D_MODEL ** -0.5),
        'w_gu_moe': nrm((no, N_EXPERTS, D_MODEL, 2 * D_FF_EXPERT), D_MODEL ** -0.5),
        'w_down_moe': nrm((no, N_EXPERTS, D_FF_EXPERT, D_MODEL), D_FF_EXPERT ** -0.5),
    }


def reference(x_prompt, x_sample, cache_a_w128, cache_a_w512, cache_a_w2048, cache_b,
              norm_mix_a, w_in_a, q_gain_a, k_gain_a, w_out_a, norm_ffn_dense, w_gu_dense, w_down_dense,
              norm_mix_b, w_in_b, q_gain_b, k_gain_b, sink_b, w_out_b, norm_ffn_moe, w_router,
              w_gu_moe, w_down_moe):
    caches_a = (cache_a_w128, cache_a_w512, cache_a_w2048)
    slopes_a = alibi_slopes(H_A)
    slopes_b = alibi_slopes(HQ_B)
    xp, xs = x_prompt, x_sample
    S = xp.shape[1]
    new_a_p = [[] for _ in A_GROUPS]
    new_a_s = [[] for _ in A_GROUPS]
    new_b_p, new_b_s = [], []
    for i in range(DEPTH):
        li = i // 2
        if i % 2 == 0:
            qp, kp, vp = qkv_dilated(rms_norm(xp, norm_mix_a[li]), w_in_a[li], q_gain_a[li], k_gain_a[li])
            qs, ks_, vs = qkv_dilated(rms_norm(xs, norm_mix_a[li]), w_in_a[li], q_gain_a[li], k_gain_a[li])
            outs_p, lses_p, outs_s, lses_s = [], [], [], []
            for g, (window, dil) in enumerate(A_GROUPS):
                n_steps = window // dil
                o, l = dilated_group_prompt(qp[:, :, g], kp[:, :, g], vp[:, :, g], dil, n_steps, slopes_a)
                outs_p.append(o)
                lses_p.append(l)
                kv_new = jnp.stack([ks_[:, :, g], vs[:, :, g]], axis=2)
                o, l = window_attention_step(qs[:, :, g], caches_a[g][li], kv_new, dil, n_steps, slopes_a, None)
                outs_s.append(o)
                lses_s.append(l)
                keep = min(window, S)
                new_a_p[g].append(jnp.stack([kp[:, S - keep:, g], vp[:, S - keep:, g]], axis=2))
                new_a_s[g].append(kv_new)
            xp = xp + out_proj(merge_by_denominator(outs_p, lses_p), w_out_a[li])
            xs = xs + out_proj(merge_by_denominator(outs_s, lses_s), w_out_a[li])
            xp = xp + swiglu(rms_norm(xp, norm_ffn_dense[li]), w_gu_dense[li], w_down_dense[li])
            xs = xs + swiglu(rms_norm(xs, norm_ffn_dense[li]), w_gu_dense[li], w_down_dense[li])
        else:
            qp, kp, vp = qkv_swa(rms_norm(xp, norm_mix_b[li]), w_in_b[li], q_gain_b[li], k_gain_b[li])
            qs, ks_, vs = qkv_swa(rms_norm(xs, norm_mix_b[li]), w_in_b[li], q_gain_b[li], k_gain_b[li])
            op, _ = banded_window_attention(qp, kp, vp, 1, WINDOW_B, slopes_b, sink_b[li])
            kv_new = jnp.stack([ks_, vs], axis=2)
            os_, _ = window_attention_step(qs, cache_b[li], kv_new, 1, WINDOW_B, slopes_b, sink_b[li])
            keep = min(WINDOW_B, S)
            new_b_p.append(jnp.stack([kp[:, S - keep:], vp[:, S - keep:]], axis=2))
            new_b_s.append(kv_new)
            xp = xp + out_proj(op, w_out_b[li])
            xs = xs + out_proj(os_, w_out_b[li])
            xp = xp + moe_swiglu(rms_norm(xp, norm_ffn_moe[li]), w_router[li], w_gu_moe[li], w_down_moe[li])
            xs = xs + moe_swiglu(rms_norm(xs, norm_ffn_moe[li]), w_router[li], w_gu_moe[li], w_down_moe[li])
    a128_prompt = jnp.stack(new_a_p[0], axis=0)
    a128_sample = jnp.stack(new_a_s[0], axis=0)
    a512_prompt = jnp.stack(new_a_p[1], axis=0)
    a512_sample = jnp.stack(new_a_s[1], axis=0)
    a2048_prompt = jnp.stack(new_a_p[2], axis=0)
    a2048_sample = jnp.stack(new_a_s[2], axis=0)
    b_prompt = jnp.stack(new_b_p, axis=0)
    b_sample = jnp.stack(new_b_s, axis=0)
    return (xp, xs, a128_prompt, a128_sample, a512_prompt, a512_sample, a2048_prompt, a2048_sample, b_prompt, b_sample)
```

```python
import numpy as np
from contextlib import ExitStack
import concourse.bass as bass
import concourse.mybir as mybir
from concourse.bass_utils import run_bass_kernel_spmd

F32 = mybir.dt.float32
BF16 = mybir.dt.bfloat16
AF = mybir.ActivationFunctionType
ALU = mybir.AluOpType
AX = mybir.AxisListType

D = 1024
KC = 8
NT = 2176
NS = 16
NC = NT + NS
NKV = 2048
NA = NKV + NC
GD = [1, 4, 16]
EPS = 1e-6
NEG = -30000.0
SLOPES = [2.0 ** (-8.0 * h / 16.0) for h in range(1, 17)]

PV_NMA, PV_NFD, PV_NMB, PV_NFM, PV_GQA, PV_GKA, PV_GQB, PV_GKB, PV_SINK, PV_N = 0, 8, 16, 24, 32, 35, 38, 39, 40, 56
C_ID, C_BD64, C_BD1, C_DNEG, C_MASK, C_ONES, C_AB, C_A0, C_A1, C_B0, C_B1, C_SEL, C_N = (
    0, 128, 256, 384, 640, 896, 1024, 1072, 1200, 1328, 1456, 1584, 1584 + 1024)


def ctiles(lo, hi, step=512):
    out = []
    c = lo
    while c < hi:
        n = min(step, hi - c)
        out.append((c, n))
        c += n
    return out


def btiles(lo, hi, step=512):
    tot = hi - lo
    nt = -(-tot // step)
    base = (tot // nt) // 2 * 2
    sizes = [base] * nt
    rem = tot - base * nt
    i = 0
    while rem > 0:
        add = min(2, rem)
        sizes[i % nt] += add
        rem -= add
        i += 1
    out = []
    c = lo
    for n in sizes:
        out.append((c, n))
        c += n
    assert c == hi and max(sizes) <= step
    return out


class Tile:
    _n = 0

    def __init__(self, name, h):
        Tile._n += 1
        self.name = "%s_%d" % (name, Tile._n)
        self.h = h
        self.w = {}
        self.pw = {}
        self.r = {}
        self.dsem = None
        self.dcnt = 0
        self.excl = False

    def seal(self):
        for k, v in self.pw.items():
            if k not in self.w or self.w[k][1] < v[1]:
                self.w[k] = v
        self.pw = {}

    def __getitem__(self, key):
        return self.h[key]


def _merge(dst, src):
    for k, v in src.items():
        if k not in dst or dst[k][1] < v[1]:
            dst[k] = v


class Sched:
    def __init__(self, nc):
        self.nc = nc
        self.E = dict(pe=nc.tensor, act=nc.scalar, dve=nc.vector, pool=nc.gpsimd, sp=nc.sync)
        self.sem = {e: nc.alloc_semaphore("s_" + e) for e in ("pe", "act", "dve", "pool")}
        self.cnt = {e: 0 for e in self.sem}
        self.seen = {e: {} for e in self.E}
        self.tiles = []
        self.nsem = 4

    def tile(self, name, shape, dt, psum=False):
        if psum:
            h = self.nc.alloc_psum_tensor("t_" + name, shape, dt)
        else:
            h = self.nc.alloc_sbuf_tensor("t_" + name, shape, dt)
        t = Tile(name, h)
        t.excl = psum
        self.tiles.append(t)
        return t

    def dram(self, name, shape, dt):
        h = self.nc.dram_tensor("t_" + name, shape, dt)
        t = Tile(name, h.ap())
        self.tiles.append(t)
        return t

    def ext(self, name, ap):
        t = Tile(name, ap)
        self.tiles.append(t)
        return t

    def _wait(self, eng, deps):
        for k, (sem, val) in deps.items():
            if k == eng and eng == "pe":
                continue
            if self.seen[eng].get(k, 0) < val:
                self.E[eng].wait_ge(sem, val)
                self.seen[eng][k] = val

    def _deps(self, reads, writes, pwrites, eng=None):
        deps = {}
        for t in reads:
            _merge(deps, t.w)
            _merge(deps, t.pw)
            if t.excl:
                _merge(deps, {k: v for k, v in t.r.items() if k != eng})
        for t in writes:
            _merge(deps, t.w)
            _merge(deps, t.pw)
            _merge(deps, t.r)
        for t in pwrites:
            _merge(deps, t.w)
            _merge(deps, t.r)
        return deps

    def _commit(self, dep, reads, writes, pwrites):
        for t in writes:
            t.w = dict(dep)
            t.pw = {}
            t.r = {}
        for t in pwrites:
            _merge(t.pw, dep)
        for t in reads:
            _merge(t.r, dep)

    def op(self, eng, fn, reads=(), writes=(), pwrites=()):
        self._wait(eng, self._deps(reads, writes, pwrites, eng))
        inst = fn(self.E[eng])
        self.cnt[eng] += 1
        inst.then_inc(self.sem[eng], 1)
        dep = {eng: (self.sem[eng], self.cnt[eng])}
        self._commit(dep, reads, writes, pwrites)

    def dma(self, q, out, in_, reads=(), writes=(), pwrites=(), owner=None):
        self._wait(q, self._deps(reads, writes, pwrites, q))
        if owner is None:
            owner = (list(writes) + list(pwrites) + list(reads))[0]
        if owner.dsem is None:
            owner.dsem = self.nc.alloc_semaphore("d_" + owner.name)
            self.nsem += 1
        owner.dcnt += 16
        self.E[q].dma_start(out=out, in_=in_).then_inc(owner.dsem, 16)
        dep = {("d", owner.name): (owner.dsem, owner.dcnt)}
        self._commit(dep, reads, writes, pwrites)

    def fence(self):
        deps = {}
        for e in self.sem:
            if self.cnt[e] > 0:
                deps[e] = (self.sem[e], self.cnt[e])
        for t in self.tiles:
            if t.dsem is not None:
                deps[("d", t.name)] = (t.dsem, t.dcnt)
        for e in self.E:
            self._wait(e, deps)


class _Stop(Exception):
    pass


def build_program(dbg=None, stop=None):
    nc = bass.Bass("TRN2", target_bir_lowering=False)
    S = Sched(nc)
    uid = {"n": 0}

    def sbt(name, shape, dt):
        uid["n"] += 1
        return nc.sbuf_tensor("%s_u%d" % (name, uid["n"]), shape, dt)

    def din(name, shape):
        return S.ext(name, nc.dram_tensor(name, list(shape), F32, kind="ExternalInput").ap())

    def dout(name, shape):
        return S.ext(name, nc.dram_tensor(name, list(shape), F32, kind="ExternalOutput").ap())

    xin = din("xin", [NA, D])
    ca = [din("ca%d" % g, [NS, 128, 2048]) for g in range(3)]
    cb = din("cb", [NS, 128, 256])
    w_in_a = din("w_in_a", [D, 9216])
    w_out_a = din("w_out_a", [D, D])
    w_gu_d = din("w_gu_dense", [D, 5632])
    w_dn_d = din("w_down_dense", [2816, D])
    w_in_b = din("w_in_b", [D, 1280])
    w_out_b = din("w_out_b", [D, D])
    w_rt = din("w_router", [D, 8])
    w_gu_m = din("w_gu_moe", [8, D, 2816])
    w_dn_m = din("w_down_moe", [8, 1408, D])
    pvec_d = din("pvec", [128, PV_N])
    cst_d = din("cst", [128, C_N])
    vld_d = din("vld", [128, 112])

    y_p = dout("y_p", [2048, D])
    y_s = dout("y_s", [NS, D])
    kvp = [dout("kvp%d" % g, [128 * GD[g], 2048]) for g in range(3)]
    kvs = [dout("kvs%d" % g, [NS, 2048]) for g in range(3)]
    bp = dout("bp", [128, 256])
    bs = dout("bs", [NS, 256])
    dbg_out = {}
    if dbg:
        for name, shape in dbg.items():
            dbg_out[name] = dout(name, shape)

    OTd = S.dram("OTd", [8, 128, NC], BF16)

    cst = S.tile("cst", [128, C_N], F32)
    pvec = S.tile("pvec", [128, PV_N], F32)
    vld = S.tile("vld", [128, 112], F32)
    cbf = S.tile("cbf", [128, 3 * 128 + 2], BF16)
    esink = S.tile("esink", [128, 16], F32)
    R1 = S.tile("R1", [128, 70144], mybir.dt.uint8)
    hA = R1.h[:, 0:KC * NA * 2].bitcast(BF16).rearrange("p (k a) -> p k a", k=KC)
    xT = R1.h[:, 0:KC * NC * 4].bitcast(F32).rearrange("p (k a) -> p k a", k=KC)
    PS = [S.tile("ps%d" % i, [128, 512], F32, psum=True) for i in range(8)]

    ident = cst.h[:, C_ID:C_ID + 128]
    identb = cbf.h[:, 0:128]
    bd64b = cbf.h[:, 128:256]
    onesb = cbf.h[:, 256:384]

    S.dma("sp", cst.h[:], cst_d.h, writes=[cst])
    S.dma("sp", pvec.h[:], pvec_d.h, writes=[pvec])
    S.dma("sp", vld.h[:], vld_d.h, writes=[vld])
    S.op("dve", lambda e: e.tensor_copy(out=cbf.h[:, 0:128], in_=cst.h[:, C_ID:C_ID + 128]), reads=[cst], pwrites=[cbf])
    S.op("dve", lambda e: e.tensor_copy(out=cbf.h[:, 128:256], in_=cst.h[:, C_BD64:C_BD64 + 128]), reads=[cst], pwrites=[cbf])
    S.op("dve", lambda e: e.tensor_copy(out=cbf.h[:, 256:384], in_=cst.h[:, C_ONES:C_ONES + 128]), reads=[cst], pwrites=[cbf])
    cbf.seal()
    S.op("act", lambda e: e.activation(out=esink.h[:], in_=pvec.h[:, PV_SINK:PV_SINK + 16], func=AF.Exp),
         reads=[pvec], writes=[esink])

    rr = {"ps": 0}
    epsb = S.tile("epsb", [128, 1], F32)
    S.op("dve", lambda e: e.memset(epsb.h[:], EPS), writes=[epsb])

    def stop_at(tag):
        if stop == tag:
            S.fence()
            raise _Stop()


    def rms_a(src_ps, n, sq):
        S.op("act", lambda e: e.activation(out=sq.h[:, 0:n], in_=src_ps.h[:, 0:n], func=AF.Square),
             reads=[src_ps], writes=[sq])

    def rms_b(src_ps, n, gain_ap, sq, sd, bank_ss, outs):
        S.op("pe", lambda e: e.matmul(bank_ss.h[:, 0:n], lhsT=bd64b, rhs=sq.h[:, 0:n], start=True, stop=True),
             reads=[sq, cbf], writes=[bank_ss])
        S.op("act", lambda e: e.activation(out=sd.h[:, 0:n], in_=bank_ss.h[:, 0:n], func=AF.Ln, bias=epsb.h[:, 0:1], scale=1.0),
             reads=[bank_ss, epsb], writes=[sd])
        S.op("act", lambda e: e.activation(out=sd.h[:, 0:n], in_=sd.h[:, 0:n], func=AF.Exp, scale=-0.5), reads=[sd], writes=[sd])
        for (lo, hi, oap, otile, pr, view) in outs:
            i0 = src_ps.h[pr, lo:hi]
            i1 = sd.h[pr, lo:hi]
            if view is not None:
                i0, i1 = view(i0), view(i1)
            S.op("dve", lambda e, oap=oap, i0=i0, i1=i1, pr=pr: e.scalar_tensor_tensor(
                out=oap, in0=i0, scalar=gain_ap[pr, :], in1=i1, op0=ALU.mult, op1=ALU.mult),
                reads=[src_ps, sd, pvec], pwrites=[otile])

    def norm_cols(src_fn, n, gcol, dst_fn, dst_tile, src_tiles, tmp, hook=None):
        sq, sd, bank = tmp
        S.op("act", lambda e: e.activation(out=sq.h[:, :, 0:n], in_=src_fn(None), func=AF.Square),
             reads=src_tiles, writes=[sq])

        def mm(e):
            for k in range(KC):
                i = e.matmul(bank.h[:, 0:n], lhsT=bd64b_all, rhs=sq.h[:, k, 0:n], start=(k == 0), stop=(k == KC - 1))
            return i
        S.op("pe", mm, reads=[sq, cbf], writes=[bank])
        S.op("act", lambda e: e.activation(out=sd.h[:, 0:n], in_=bank.h[:, 0:n], func=AF.Ln, bias=epsb.h[:, 0:1], scale=1.0),
             reads=[bank, epsb], writes=[sd])
        S.op("act", lambda e: e.activation(out=sd.h[:, 0:n], in_=sd.h[:, 0:n], func=AF.Exp, scale=-0.5), reads=[sd], writes=[sd])
        for k in range(KC):
            S.op("dve", lambda e, k=k: e.scalar_tensor_tensor(out=dst_fn(k), in0=src_fn(k), scalar=pvec.h[:, gcol + k:gcol + k + 1],
                                                               in1=sd.h[:, 0:n], op0=ALU.mult, op1=ALU.mult),
                 reads=src_tiles + [sd, pvec], pwrites=[dst_tile])
            if hook is not None:
                hook(k, sd)

    bdall = S.tile("bdall", [128, 128], BF16)
    S.op("act", lambda e: e.activation(out=bdall.h[:], in_=cst.h[:, C_ONES:C_ONES + 128], func=AF.Copy, scale=1.0 / 1024.0),
         reads=[cst], writes=[bdall])
    bd64b_all = bdall.h[:, :]

    def load_x_cols(a0, n, dst_fn, dst_tile, xr, bank2):
        nsub = (n + 127) // 128
        for i in range(nsub):
            nr = min(128, n - 128 * i)
            S.dma("sp", xr[i].h[0:nr, :], xin.h[a0 + 128 * i:a0 + 128 * i + nr, :], writes=[xr[i]])
        for k in range(KC):
            bank = bank2[k % 2]

            def tr(e, k=k, bank=bank):
                for i in range(nsub):
                    nr = min(128, n - 128 * i)
                    ins = e.transpose(bank.h[:, 128 * i:128 * i + nr], xr[i].h[0:nr, k * 128:(k + 1) * 128], ident[0:nr, 0:nr])
                return ins
            S.op("pe", tr, reads=[xr[i] for i in range(nsub)] + [cst], writes=[bank])
            S.op("act", lambda e, k=k, bank=bank: e.activation(out=dst_fn(k), in_=bank.h[:, 0:n], func=AF.Copy),
                 reads=[bank], pwrites=[dst_tile])

    with sbt("p0_xt", [128, KC, 512], F32) as xt_h, \
            sbt("p0_sq", [128, KC, 512], BF16) as sq_h, \
            sbt("p0_sd", [128, 512], F32) as sd_h, \
            sbt("p0_xr", [128, 4, D], F32) as xr_h:
        xt = Tile("p0_xt", xt_h)
        sq = Tile("p0_sq", sq_h)
        sd = Tile("p0_sd", sd_h)
        xr = [Tile("p0_xr%d" % i, xr_h[:, i, :]) for i in range(4)]
        S.tiles += [xt, sq, sd] + xr
        for (a0, n) in ctiles(0, NA):
            load_x_cols(a0, n, lambda k: xt.h[:, k, 0:n], xt, xr, (PS[0], PS[1]))
            xt.seal()
            norm_cols(lambda k: xt.h[:, :, 0:n] if k is None else xt.h[:, k, 0:n], n, PV_NMA,
                      lambda k: hA[:, k, a0:a0 + n], R1, [xt], (sq, sd, PS[2]))
        R1.seal()
        S.fence()
    if dbg and "d_hA" in dbg:
        with sbt("dbgt", [128, KC, NA], F32) as dt_h:
            dtile = Tile("dbgt", dt_h)
            S.tiles.append(dtile)
            S.op("dve", lambda e: e.tensor_copy(out=dt_h[:], in_=hA[:, :, :]), reads=[R1], writes=[dtile])
            S.dma("sp", dbg_out["d_hA"].h.rearrange("(p k) a -> p k a", p=128), dt_h[:], reads=[dtile])
            S.fence()

    def attention_layer(layer, hsrc, hoff, htile):
        isA = layer == 0
        groups = [0, 1, 2] if isA else [0]
        hist = 1 if isA else 0
        with ExitStack() as es_:
            w_h = es_.enter_context(sbt("a_w", [128, 2, 3, KC, 128], BF16))
            qT_h = es_.enter_context(sbt("a_qT", [128, NS], BF16))
            qz_h = es_.enter_context(sbt("a_qz", [128, 2, NC], BF16))
            kT_h = es_.enter_context(sbt("a_kT", [128, NA], BF16))
            kTf_h = es_.enter_context(sbt("a_kTf", [128, NC], F32))
            V_h = es_.enter_context(sbt("a_V", [128, 48, 2, 128], BF16))
            acc_h = es_.enter_context(sbt("a_acc", [128, 2, NC], F32))
            bias_h = es_.enter_context(sbt("a_bias", [128, 3, 2, 2, 128], F32))
            sb_h = es_.enter_context(sbt("a_et", [128, 4, 2, 2, 128], BF16))
            eb_h = es_.enter_context(sbt("a_eb", [128, 3, 2, 2, 128], BF16))
            pT_h = es_.enter_context(sbt("a_pT", [128, 4, 2, 2, 128], BF16))
            sq_h = es_.enter_context(sbt("a_sq", [128, 3, 512], BF16))
            sd_h = es_.enter_context(sbt("a_sd", [128, 2, 512], F32))
            ohp_h = es_.enter_context(sbt("a_ohp", [128, NC], BF16))
            vf_h = es_.enter_context(sbt("a_vf", [128, 3, 128], F32))
            kc_h = es_.enter_context(sbt("a_kc", [128, NS, 2, 64], BF16))
            vc_h = es_.enter_context(sbt("a_vc", [128, NS, 2, 64], BF16))
            qd_h = es_.enter_context(sbt("a_qd", [128, NS, 128], BF16))
            prod_h = es_.enter_context(sbt("a_prod", [128, 512], F32))
            p2_h = es_.enter_context(sbt("a_p2", [128, NS // 2, 2, 128], BF16))
            sm_h = es_.enter_context(sbt("a_sm", [128, 256], F32))
            wT = [[Tile("a_w%d%d" % (b, j), w_h[:, b, j]) for j in range(3)] for b in range(2)]
            qT = Tile("a_qT", qT_h)
            qz = Tile("a_qz", qz_h)
            S.tiles.append(qz)
            S.op("dve", lambda e: e.memset(qz_h[:], 0.0), writes=[qz])
            kT = Tile("a_kT", kT_h)
            kTf = Tile("a_kTf", kTf_h)
            Vt = Tile("a_V", V_h)
            acc = Tile("a_acc", acc_h)
            biasT = Tile("a_bias", bias_h)
            ebT = Tile("a_eb", eb_h)
            S.tiles.append(ebT)
            sbT = [Tile("a_sb%d" % i, sb_h[:, i]) for i in range(4)]
            pTT = [Tile("a_pT%d" % i, pT_h[:, i]) for i in range(4)]
            sqT = [Tile("a_sq%d" % i, sq_h[:, i]) for i in range(3)]
            sdT = [Tile("a_sd%d" % i, sd_h[:, i]) for i in range(2)]
            ohp = Tile("a_ohp", ohp_h)
            vf = [Tile("a_vf%d" % i, vf_h[:, i]) for i in range(3)]
            kc = Tile("a_kc", kc_h)
            vc = Tile("a_vc", vc_h)
            qd = Tile("a_qd", qd_h)
            prod = Tile("a_prod", prod_h)
            p2 = Tile("a_p2", p2_h)
            sm = Tile("a_sm", sm_h)
            newt = [qT, kT, kTf, Vt, acc, biasT, ohp, kc, vc, qd, prod, p2, sm] + sqT + sdT + sbT + pTT + vf + wT[0] + wT[1]
            S.tiles += newt
            bq = [PS[3], PS[4]]
            bss = PS[2]
            bsc = [PS[3], PS[4], PS[2]]
            bo = [PS[5], PS[6]]
            bx = PS[7]
            cnt = {"w": 0, "u": 0, "vf": 0}
            w_in = w_in_a if isA else w_in_b

            def wview(c0, ncols=128):
                return w_in.h[:, c0:c0 + ncols].rearrange("(k p) c -> p k c", p=128)

            its = [(hp_, g_) for hp_ in range(8) for g_ in groups]

            def issue_w(idx):
                hp_, g_ = its[idx]
                wb_ = wT[idx % 2]
                if isA:
                    cq = g_ * 3072 + hp_ * 128
                    S.dma("pool", wb_[0].h[:], wview(cq), writes=[wb_[0]])
                    S.dma("pool", wb_[1].h[:], wview(cq + 1024), writes=[wb_[1]])
                    S.dma("pool", wb_[2].h[:], wview(cq + 2048), writes=[wb_[2]])
                else:
                    S.dma("pool", wb_[0].h[:], wview(hp_ * 128), writes=[wb_[0]])
                    if hp_ % 4 == 0:
                        kvh_ = hp_ // 4
                        for hh in range(2):
                            S.dma("pool", wb_[1].h[:, :, hh * 64:(hh + 1) * 64], wview(1024 + kvh_ * 64, 64), pwrites=[wb_[1]])
                            S.dma("pool", wb_[2].h[:, :, hh * 64:(hh + 1) * 64], wview(1152 + kvh_ * 64, 64), pwrites=[wb_[2]])
                        wb_[1].seal()
                        wb_[2].seal()

            for hp in range(8):
                for g in groups:
                    d = GD[g]
                    a0 = hoff - 128 * d * hist
                    Lk = hoff + NC - a0
                    U = NT // d
                    nb = (U + 127) // 128 + hist
                    voff = [0, 18, 42][g] if isA else 90
                    do_kv = isA or (hp % 4 == 0)
                    wb = wT[cnt["w"] % 2]
                    if cnt["w"] == 0:
                        issue_w(0)
                    if cnt["w"] + 1 < len(its):
                        issue_w(cnt["w"] + 1)
                    cnt["w"] += 1
                    stop_at('a%d_w' % layer)
                    gq = pvec.h[:, PV_GQA + g:PV_GQA + g + 1] if isA else pvec.h[:, PV_GQB:PV_GQB + 1]
                    gk = pvec.h[:, PV_GKA + g:PV_GKA + g + 1] if isA else pvec.h[:, PV_GKB:PV_GKB + 1]
                    W = 128 * d

                    ALLP = slice(0, 128)
                    UK = U + 128 * hist

                    def project(items):
                        bq4 = [PS[0], PS[1], PS[5], PS[6]]

                        def stage_a(i):
                            wt, col0, gain, outs_fn, c0, n = items[i]
                            bank = bq4[i % 4]

                            def mm(e):
                                for k in range(KC):
                                    ins = e.matmul(bank.h[:, 0:n], lhsT=wt.h[:, k, :], rhs=hsrc[:, k, col0 + c0:col0 + c0 + n],
                                                   start=(k == 0), stop=(k == KC - 1))
                                return ins
                            S.op("pe", mm, reads=[wt, htile], writes=[bank])
                            rms_a(bank, n, sqT[i % 3])

                        def stage_b(i):
                            wt, col0, gain, outs_fn, c0, n = items[i]
                            rms_b(bq4[i % 4], n, gain, sqT[i % 3], sdT[i % 2], bss, outs_fn(c0, n))
                        AH = 2
                        for i in range(min(AH, len(items))):
                            stage_a(i)
                        for i in range(len(items)):
                            if i + AH < len(items):
                                stage_a(i + AH)
                            stage_b(i)

                    def deint(ap2d):
                        return ap2d.rearrange("p (u r) -> p u r", r=d)

                    def q_outs(c0, n):
                        outs = []
                        hi = min(c0 + n, NT)
                        if c0 < hi:
                            u0, nu = c0 // d, (hi - c0) // d
                            for hh in range(2):
                                pr = slice(hh * 64, (hh + 1) * 64)
                                oap = qz.h[pr, hh, 0:NT].rearrange("p (r u) -> p u r", r=d)[:, u0:u0 + nu, :]
                                outs.append((0, hi - c0, oap, qz, pr, deint))
                        lo, hi = max(c0, NT), min(c0 + n, NC)
                        if lo < hi:
                            outs.append((lo - c0, hi - c0, qT.h[:, lo - NT:hi - NT], qT, ALLP, None))
                        return outs

                    def k_outs(c0, n):
                        outs = []
                        hi = min(c0 + n, UK * d)
                        if c0 < hi:
                            outs.append((0, hi - c0, kT.h[:, c0:hi], kT, ALLP, None))
                        wlo, whi = Lk - NS - W, Lk - NS
                        lo, hi = max(c0, wlo), min(c0 + n, whi)
                        if lo < hi:
                            outs.append((lo - c0, hi - c0, kTf.h[:, lo - wlo:hi - wlo], kTf, ALLP, None))
                        lo, hi = max(c0, Lk - NS), min(c0 + n, Lk)
                        if lo < hi:
                            outs.append((lo - c0, hi - c0, kTf.h[:, 2048 + lo - (Lk - NS):2048 + hi - (Lk - NS)], kTf, ALLP, None))
                        return outs

                    items = [(wb[0], hoff, gq, q_outs, c0, n) for (c0, n) in ctiles(0, NC)]
                    if do_kv:
                        items += [(wb[1], a0, gk, k_outs, c0, n) for (c0, n) in ctiles(0, Lk)]
                    project(items)
                    qz.seal()
                    qT.seal()
                    if do_kv:
                        kT.seal()
                        kTf.seal()
                    stop_at('a%d_k' % layer)

                    if do_kv:
                        ntile = nb * d
                        S.op("act", lambda e, ntile=ntile, voff=voff: e.activation(
                            out=Vt.h[:, 0:ntile, :, 64:128],
                            in_=vld.h[:, voff:voff + ntile].unsqueeze(2).unsqueeze(3).to_broadcast([128, ntile, 2, 64]),
                            func=AF.Copy), reads=[vld], pwrites=[Vt])
                        tl = [(r, bb) for r in range(d) for bb in range(nb)]
                        for t0 in range(0, ntile, 4):
                            grp = tl[t0:t0 + 4]
                            bank = bq[rr["ps"] % 2]
                            rr["ps"] += 1

                            def mmv(e, grp=grp, bank=bank):
                                for j, (r, bb) in enumerate(grp):
                                    u0 = 128 * (bb - hist)
                                    nk = min(128, U - u0) if u0 >= 0 else 128
                                    a1 = hoff + u0 * d + r
                                    for k in range(KC):
                                        i = e.matmul(bank.h[0:nk, j * 128:(j + 1) * 128],
                                                     lhsT=hsrc[:, k, a1:a1 + (nk - 1) * d + 1:d], rhs=wb[2].h[:, k, :],
                                                     start=(k == 0), stop=(k == KC - 1))
                                return i
                            S.op("pe", mmv, reads=[wb[2], htile], writes=[bank])
                            ng = len(grp)
                            S.op("dve", lambda e, t0=t0, ng=ng, bank=bank, voff=voff: e.tensor_tensor(
                                out=Vt.h[:, t0:t0 + ng, :, 0:64],
                                in0=bank.h[:, 0:ng * 128].rearrange("p (t h x) -> p t h x", t=ng, h=2),
                                in1=vld.h[:, voff + t0:voff + t0 + ng].unsqueeze(2).unsqueeze(3).to_broadcast([128, ng, 2, 64]),
                                op=ALU.mult), reads=[bank, vld], pwrites=[Vt])
                            if isA or hp % 4 == 0:
                                for j, (r, bb) in enumerate(grp):
                                    u0 = 128 * (bb - hist)
                                    if u0 < 0:
                                        continue
                                    nk = min(128, U - u0)
                                    i0 = max(0, -(-(NT - W - r) // d) - u0)
                                    if i0 >= nk:
                                        continue
                                    vft = vf[cnt["vf"] % 3]
                                    cnt["vf"] += 1
                                    S.op("act", lambda e, j=j, i0=i0, nk=nk, vft=vft, bank=bank: e.activation(
                                        out=vft.h[0:nk, :], in_=bank.h[0:nk, j * 128:(j + 1) * 128], func=AF.Copy),
                                        reads=[bank], writes=[vft])
                                    row0 = (u0 + i0) * d + r - (NT - W)
                                    cnt_rows = nk - i0
                                    if isA:
                                        dst = kvp[g].h[row0:row0 + (cnt_rows - 1) * d + 1:d, 1024 + hp * 128:1024 + (hp + 1) * 128]
                                        S.dma("sp", dst, vft.h[i0:nk, :], reads=[vft], pwrites=[kvp[g]], owner=vft)
                                    else:
                                        kvh = hp // 4
                                        dst = bp.h[row0:row0 + cnt_rows, 128 + kvh * 64:128 + (kvh + 1) * 64]
                                        S.dma("sp", dst, vft.h[i0:nk, 0:64], reads=[vft], pwrites=[bp], owner=vft)
                        Vt.seal()
                        stop_at('a%d_v' % layer)

                    def ktr_gen():
                        if do_kv:
                            for j in range(d):
                                S.op("pe", lambda e, j=j: e.transpose(PS[0].h[:, 0:128], kTf.h[:, j * 128:(j + 1) * 128], ident),
                                     reads=[kTf, cst], writes=[PS[0]])
                                vft = vf[cnt["vf"] % 3]
                                cnt["vf"] += 1
                                S.op("act", lambda e, vft=vft: e.activation(out=vft.h[:, :], in_=PS[0].h[:, 0:128], func=AF.Copy),
                                     reads=[PS[0]], writes=[vft])
                                if isA:
                                    S.dma("sp", kvp[g].h[j * 128:(j + 1) * 128, hp * 128:(hp + 1) * 128], vft.h[:, :],
                                          reads=[vft], pwrites=[kvp[g]], owner=vft)
                                else:
                                    kvh = hp // 4
                                    S.dma("sp", bp.h[:, kvh * 64:(kvh + 1) * 64], vft.h[:, 0:64], reads=[vft], pwrites=[bp], owner=vft)
                                yield
                        yield

                    stop_at('a%d_qkv' % layer)
                    gi = g if isA else 0
                    for hh in range(2):
                        md = SLOPES[2 * hp + hh] * d
                        S.op("dve", lambda e, hh=hh, md=md, gi=gi: e.scalar_tensor_tensor(
                            out=biasT.h[:, gi, :, hh, :], in0=cst.h[:, C_DNEG:C_DNEG + 256].rearrange("p (b x) -> p b x", b=2), scalar=float(md),
                            in1=cst.h[:, C_MASK:C_MASK + 256].rearrange("p (b x) -> p b x", b=2), op0=ALU.mult, op1=ALU.add),
                            reads=[cst], pwrites=[biasT])
                    biasT.seal()
                    S.op("act", lambda e: e.activation(out=ebT.h[:, gi], in_=biasT.h[:, gi], func=AF.Exp), reads=[biasT], writes=[ebT])

                    nqb = (U + 127) // 128
                    units = [(r, B) for r in range(d) for B in range(1 - hist, nqb)]

                    def unit_geom(r, B):
                        nq = min(128, U - 128 * B)
                        tp = r * nb + B - 1 + hist
                        qc0 = 128 * B * d + r
                        qsl = slice(qc0, qc0 + (nq - 1) * d + 1, d)
                        qcs = slice(r * U + 128 * B, r * U + 128 * B + nq)
                        kp0 = 128 * (B - 1 + hist) * d + r
                        ksl_p = slice(kp0, kp0 + 127 * d + 1, d)
                        kc0 = kp0 + 128 * d
                        ksl_c = slice(kc0, kc0 + (nq - 1) * d + 1, d)
                        return nq, tp, tp + 1, qsl, ksl_p, ksl_c, qcs

                    def unit_scores(ui):
                        r, B = units[ui]
                        nq, tp, tcur, qsl, ksl_p, ksl_c, qcs = unit_geom(r, B)
                        bs_ = bsc[ui % 3]
                        sbt = sbT[ui % 4]
                        ptt = pTT[ui % 4]

                        bv = bs_.h[:, :].rearrange("p (b h x) -> p b h x", b=2, h=2)

                        def mms(e):
                            if nq == 128:
                                e.matmul(bs_.h[:, 0:256], lhsT=kT.h[:, ksl_p], rhs=qz.h[:, :, qcs], start=True, stop=True)
                                return e.matmul(bs_.h[:, 256:512], lhsT=kT.h[:, ksl_c], rhs=qz.h[:, :, qcs], start=True, stop=True)
                            for hh in range(2):
                                e.matmul(bv[:, 0, hh, 0:nq], lhsT=kT.h[:, ksl_p], rhs=qz.h[:, hh, qcs], start=True, stop=True)
                                i = e.matmul(bv[0:nq, 1, hh, 0:nq], lhsT=kT.h[:, ksl_c], rhs=qz.h[:, hh, qcs], start=True, stop=True)
                            return i
                        S.op("pe", mms, reads=[kT, qz], writes=[bs_])
                        if nq == 128:
                            S.op("act", lambda e: e.activation(out=sbt.h[:, :, :, :], in_=bv, func=AF.Exp, scale=0.125),
                                 reads=[bs_], writes=[sbt])
                            S.op("dve", lambda e: e.tensor_tensor(out=ptt.h[:, :, :, :], in0=sbt.h[:, :, :, :], in1=ebT.h[:, gi, :, :, :], op=ALU.mult),
                                 reads=[sbt, ebT], writes=[ptt])
                        else:
                            S.op("act", lambda e: e.activation(out=sbt.h[:, 0, :, 0:nq], in_=bv[:, 0, :, 0:nq], func=AF.Exp, scale=0.125),
                                 reads=[bs_], writes=[sbt])
                            S.op("act", lambda e: e.activation(out=sbt.h[0:nq, 1, :, 0:nq], in_=bv[0:nq, 1, :, 0:nq], func=AF.Exp, scale=0.125),
                                 reads=[bs_], pwrites=[sbt])
                            sbt.seal()
                            S.op("dve", lambda e: e.tensor_tensor(out=ptt.h[:, 0, :, 0:nq], in0=sbt.h[:, 0, :, 0:nq], in1=ebT.h[:, gi, 0, :, 0:nq], op=ALU.mult),
                                 reads=[sbt, ebT], writes=[ptt])
                            S.op("dve", lambda e: e.tensor_tensor(out=ptt.h[0:nq, 1, :, 0:nq], in0=sbt.h[0:nq, 1, :, 0:nq], in1=ebT.h[0:nq, gi, 1, :, 0:nq], op=ALU.mult),
                                 reads=[sbt, ebT], pwrites=[ptt])
                            ptt.seal()

                    def unit_pv(ui):
                        r, B = units[ui]
                        nq, tp, tcur, qsl, ksl_p, ksl_c, qcs = unit_geom(r, B)
                        bo_ = bo[ui % 2]
                        ptt = pTT[ui % 4]

                        def mmo(e):
                            for hh in range(2):
                                e.matmul(bo_.h[:, hh * 128:hh * 128 + nq], lhsT=Vt.h[:, tp, hh, :], rhs=ptt.h[:, 0, hh, 0:nq],
                                         start=True, stop=False)
                                i = e.matmul(bo_.h[:, hh * 128:hh * 128 + nq], lhsT=Vt.h[0:nq, tcur, hh, :],
                                             rhs=ptt.h[0:nq, 1, hh, 0:nq], start=False, stop=True)
                            return i
                        S.op("pe", mmo, reads=[Vt, ptt], writes=[bo_])
                        bov = bo_.h[:, 0:256].rearrange("p (h x) -> p h x", h=2)[:, :, 0:nq]
                        if g == 0:
                            S.op("act", lambda e: e.activation(out=acc.h[:, :, qsl], in_=bov, func=AF.Copy),
                                 reads=[bo_], pwrites=[acc])
                        else:
                            S.op("dve", lambda e: e.tensor_tensor(out=acc.h[:, :, qsl], in0=acc.h[:, :, qsl], in1=bov, op=ALU.add),
                                 reads=[bo_], pwrites=[acc])

                    def decode_gen():
                        csrc = ca[g] if isA else cb
                        if do_kv:
                            if isA:
                                S.dma("pool", kc.h[:].rearrange("p n h x -> p n (h x)"),
                                      csrc.h[:, :, hp * 128:(hp + 1) * 128].rearrange("n r c -> r n c"), writes=[kc])
                                yield
                                S.dma("pool", vc.h[:].rearrange("p n h x -> p n (h x)"),
                                      csrc.h[:, :, 1024 + hp * 128:1024 + (hp + 1) * 128].rearrange("n r c -> r n c"), writes=[vc])
                                yield
                            else:
                                kvh = hp // 4
                                for hh in range(2):
                                    S.dma("pool", kc.h[:, :, hh, :], csrc.h[:, :, kvh * 64:(kvh + 1) * 64].rearrange("n r c -> r n c"), pwrites=[kc])
                                    yield
                                    S.dma("pool", vc.h[:, :, hh, :], csrc.h[:, :, 128 + kvh * 64:128 + (kvh + 1) * 64].rearrange("n r c -> r n c"), pwrites=[vc])
                                    yield
                                kc.seal()
                                vc.seal()
                        S.op("dve", lambda e: e.tensor_tensor(
                            out=qd.h[:, :, :], in0=identb.unsqueeze(1).to_broadcast([128, NS, 128]),
                            in1=qT.h[:, 0:NS].unsqueeze(2).to_broadcast([128, NS, 128]), op=ALU.mult), reads=[cbf, qT], writes=[qd])
                        yield
                        for j in range(4):
                            S.op("pe", lambda e, j=j: e.matmul(bx.h[:, :], lhsT=onesb, rhs=qd.h[:, 4 * j:4 * j + 4, :].rearrange("p n c -> p (n c)"),
                                                               start=True, stop=True), reads=[qd, cbf], writes=[bx])
                            yield
                            S.op("dve", lambda e, j=j: e.tensor_tensor(out=prod.h[:, :], in0=kc.h[:, 4 * j:4 * j + 4, :, :].rearrange("p n h x -> p (n h x)"),
                                                                      in1=bx.h[:, :], op=ALU.mult), reads=[kc, bx], writes=[prod])
                            yield
                            S.op("dve", lambda e, j=j: e.tensor_reduce(out=sm.h[:, 8 * j:8 * j + 8], in_=prod.h[:, :].rearrange("p (a x) -> p a x", x=64),
                                                                      axis=AX.X, op=ALU.add), reads=[prod], pwrites=[sm])
                            yield
                        sm.seal()
                        abv = cst.h[:, C_AB + gi * 16 + 2 * hp:C_AB + gi * 16 + 2 * hp + 2].unsqueeze(1).to_broadcast([128, NS, 2])
                        S.op("dve", lambda e, abv=abv: e.scalar_tensor_tensor(
                            out=sm.h[:, 0:32].rearrange("p (n h) -> p n h", h=2), in0=sm.h[:, 0:32].rearrange("p (n h) -> p n h", h=2),
                            scalar=0.125, in1=abv, op0=ALU.mult, op1=ALU.add), reads=[sm, cst], writes=[sm])
                        yield
                        S.op("act", lambda e: e.activation(out=sm.h[:, 32:64], in_=sm.h[:, 0:32], func=AF.Exp), reads=[sm], writes=[sm])
                        yield
                        HN = NS // 2
                        for half in range(2):
                            n0 = half * HN
                            pv4 = sm.h[:, 32 + 2 * n0:32 + 2 * (n0 + HN)].rearrange("p (n h) -> p n h", h=2).unsqueeze(3).to_broadcast([128, HN, 2, 64])
                            S.op("dve", lambda e, pv4=pv4, n0=n0: e.tensor_tensor(out=p2.h[:, :, :, 0:64], in0=vc.h[:, n0:n0 + HN, :, :], in1=pv4, op=ALU.mult),
                                 reads=[vc, sm], pwrites=[p2])
                            yield
                            S.op("act", lambda e, pv4=pv4: e.activation(out=p2.h[:, :, :, 64:128], in_=pv4, func=AF.Copy), reads=[sm], pwrites=[p2])
                            yield
                            p2.seal()

                            def mmd(e, n0=n0):
                                for hh in range(2):
                                    for n in range(HN):
                                        i = e.matmul(bx.h[:, hh * NS + n0 + n:hh * NS + n0 + n + 1], lhsT=p2.h[:, n, hh, :], rhs=onesb[:, 0:1], start=True, stop=True)
                                return i
                            S.op("pe", mmd, reads=[p2, cbf], writes=[bx])
                            yield
                        bxv = bx.h[:, 0:2 * NS].rearrange("p (h n) -> p h n", h=2)
                        if g == 0:
                            S.op("dve", lambda e, bxv=bxv: e.tensor_copy(out=acc.h[:, :, NT:NC], in_=bxv), reads=[bx], pwrites=[acc])
                            yield
                        else:
                            S.op("dve", lambda e, bxv=bxv: e.tensor_tensor(out=acc.h[:, :, NT:NC], in0=acc.h[:, :, NT:NC], in1=bxv, op=ALU.add),
                                 reads=[bx], pwrites=[acc])
                            yield
                        acc.seal()
                        if do_kv:
                            def mmvs(e):
                                for k in range(KC):
                                    i = e.matmul(bx.h[:, 64:64 + NS], lhsT=wb[2].h[:, k, :], rhs=hsrc[:, k, hoff + NT:hoff + NC], start=(k == 0), stop=(k == KC - 1))
                                return i
                            S.op("pe", mmvs, reads=[wb[2], htile], writes=[bx])
                            yield
                            S.op("act", lambda e: e.activation(out=sm.h[:, 112:128], in_=bx.h[:, 64:64 + NS], func=AF.Copy), reads=[bx], writes=[sm])
                            yield
                            S.op("dve", lambda e: e.tensor_copy(out=sm.h[:, 128:144], in_=kTf.h[:, 2048:2048 + NS]), reads=[kTf], writes=[sm])
                            yield
                        S.op("dve", lambda e: e.tensor_tensor(out=sm.h[:, 64:80], in0=qT.h[:, 0:NS], in1=sm.h[:, 128:144], op=ALU.mult),
                             reads=[qT, sm], writes=[sm])
                        yield
                        S.op("pe", lambda e: e.matmul(bx.h[:, 0:NS], lhsT=cst.h[:, C_BD1:C_BD1 + 128], rhs=sm.h[:, 64:80], start=True, stop=True),
                             reads=[sm, cst], writes=[bx])
                        yield
                        S.op("act", lambda e: e.activation(out=sm.h[:, 80:96], in_=bx.h[:, 0:NS], func=AF.Exp, scale=0.125), reads=[bx], writes=[sm])
                        yield
                        S.op("dve", lambda e: e.tensor_tensor(out=sm.h[:, 96:112], in0=sm.h[:, 112:128], in1=sm.h[:, 80:96], op=ALU.mult),
                             reads=[sm], writes=[sm])
                        yield

                        def mmn(e):
                            for hh in range(2):
                                ca_, cb_ = (C_A0, C_B0) if hh == 0 else (C_A1, C_B1)
                                e.matmul(bx.h[:, hh * NS:(hh + 1) * NS], lhsT=cst.h[:, ca_:ca_ + 128], rhs=sm.h[:, 96:112], start=True, stop=False)
                                i = e.matmul(bx.h[:, hh * NS:(hh + 1) * NS], lhsT=cst.h[:, cb_:cb_ + 128], rhs=sm.h[:, 80:96], start=False, stop=True)
                            return i
                        S.op("pe", mmn, reads=[sm, cst], writes=[bx])
                        yield
                        S.op("dve", lambda e, bxv=bxv: e.tensor_tensor(out=acc.h[:, :, NT:NC], in0=acc.h[:, :, NT:NC], in1=bxv, op=ALU.add),
                             reads=[bx], pwrites=[acc])
                        yield
                        acc.seal()
                        if do_kv and (isA or hp % 4 == 0):
                            for which, col in ((0, 128), (1, 112)):
                                S.op("pe", lambda e, col=col: e.transpose(bx.h[0:NS, 128:256], sm.h[:, col:col + NS], ident), reads=[sm, cst], writes=[bx])
                                yield
                                S.op("act", lambda e, which=which: e.activation(out=prod.h[0:NS, which * 128:(which + 1) * 128],
                                                                               in_=bx.h[0:NS, 128:256], func=AF.Copy), reads=[bx], writes=[prod])
                                yield
                                if isA:
                                    S.dma("sp", kvs[g].h[:, which * 1024 + hp * 128:which * 1024 + (hp + 1) * 128], prod.h[0:NS, which * 128:(which + 1) * 128],
                                          reads=[prod], pwrites=[kvs[g]], owner=prod)
                                    yield
                                else:
                                    kvh = hp // 4
                                    S.dma("sp", bs.h[:, which * 128 + kvh * 64:which * 128 + (kvh + 1) * 64], prod.h[0:NS, which * 128:which * 128 + 64],
                                          reads=[prod], pwrites=[bs], owner=prod)
                                    yield
                        yield

                    dg = decode_gen()
                    kg = ktr_gen()
                    PD = 3
                    for ui in range(len(units) + PD):
                        if ui < len(units):
                            unit_scores(ui)
                        if ui >= PD:
                            unit_pv(ui - PD)
                        next(dg, None)
                        next(dg, None)
                        if ui % 2 == 1:
                            next(kg, None)
                    for _ in dg:
                        pass
                    for _ in kg:
                        pass
                    acc.seal()

                stop_at('a%d_dec' % layer)
                nlo = 0 if isA else 128
                tden = kTf
                td = tden.h[0:64, nlo:NC]
                for hh in range(2):
                    if isA:
                        S.op("dve", lambda e, hh=hh: e.tensor_scalar(out=td, in0=acc.h[64:128, hh, nlo:NC], scalar1=1e-18, scalar2=None, op0=ALU.max),
                             reads=[acc], writes=[tden])
                    else:
                        S.op("dve", lambda e, hh=hh: e.tensor_copy(out=td, in_=acc.h[64:128, hh, nlo:NC]), reads=[acc], writes=[tden])
                        S.op("dve", lambda e, hh=hh: e.tensor_scalar(out=td, in0=td, scalar1=esink.h[0:64, 2 * hp + hh:2 * hp + hh + 1],
                                                                    scalar2=1e-18, op0=ALU.add, op1=ALU.max), reads=[tden, esink], writes=[tden])
                    S.op("act", lambda e: e.activation(out=td, in_=td, func=AF.Ln), reads=[tden], writes=[tden])
                    S.op("act", lambda e: e.activation(out=td, in_=td, func=AF.Exp, scale=-1.0), reads=[tden], writes=[tden])
                    if hh == 0:
                        S.op("dve", lambda e: e.tensor_tensor(out=ohp.h[0:64, nlo:NC], in0=acc.h[0:64, 0, nlo:NC], in1=td, op=ALU.mult),
                             reads=[acc, tden], pwrites=[ohp])
                    else:
                        S.op("dve", lambda e: e.tensor_tensor(out=td, in0=acc.h[0:64, 1, nlo:NC], in1=td, op=ALU.mult),
                             reads=[acc, tden], writes=[tden])
                        S.op("dve", lambda e: e.tensor_copy(out=ohp.h[64:128, nlo:NC], in_=td), reads=[tden], pwrites=[ohp])
                ohp.seal()
                stop_at('a%d_norm' % layer)
                S.dma("sp", OTd.h[hp, :, nlo:NC], ohp.h[:, nlo:NC], reads=[ohp], pwrites=[OTd], owner=ohp)
            OTd.seal()
            S.fence()

    def out_proj(layer, clo):
        coltiles = ctiles(clo, NC) if clo == 0 else btiles(clo, NC)
        w_out = w_out_a if layer == 0 else w_out_b
        with ExitStack() as es_:
            w_h = es_.enter_context(sbt("o_w", [128, KC, D], BF16))
            ot_h = es_.enter_context(sbt("o_ot", [128, 2, KC, 512], BF16))
            xr_h = es_.enter_context(sbt("o_xr", [128, 4, D], F32))
            wt = Tile("o_w", w_h)
            ott = [Tile("o_ot%d" % i, ot_h[:, i]) for i in range(2)]
            xr = [Tile("o_xr%d" % i, xr_h[:, i, :]) for i in range(4)]
            S.tiles += [wt] + ott + xr
            for half in range(2):
                S.dma("pool", wt.h[:, :, half * 512:(half + 1) * 512],
                      w_out.h[:, half * 512:(half + 1) * 512].rearrange("(k p) c -> p k c", p=128), pwrites=[wt])
            wt.seal()
            for ti, (c0, n) in enumerate(coltiles):
                ot = ott[ti % 2]
                S.dma("sp", ot.h[:, :, 0:n], OTd.h[:, :, c0:c0 + n].rearrange("h p c -> p h c"), reads=[OTd], writes=[ot])
                if layer == 0:
                    load_x_cols(NKV + c0, n, lambda k: xT[:, k, c0:c0 + n], R1, xr, (PS[0], PS[1]))
                    R1.seal()
                else:
                    S.dma("sp", xT[:, :, c0:c0 + n], xTd.h.rearrange("p (k a) -> p k a", k=KC)[:, :, c0:c0 + n], reads=[xTd], pwrites=[R1], owner=R1)
                    R1.seal()
                for dc in range(KC):
                    bank = PS[2 + dc % 2]

                    def mm(e, dc=dc, bank=bank, ot=ot, n=n):
                        for hp in range(8):
                            i = e.matmul(bank.h[:, 0:n], lhsT=wt.h[:, hp, dc * 128:(dc + 1) * 128], rhs=ot.h[:, hp, 0:n], start=(hp == 0), stop=(hp == 7))
                        return i
                    S.op("pe", mm, reads=[wt, ot], writes=[bank])
                    S.op("dve", lambda e, dc=dc, bank=bank, c0=c0, n=n: e.tensor_tensor(
                        out=xT[:, dc, c0:c0 + n], in0=xT[:, dc, c0:c0 + n], in1=bank.h[:, 0:n], op=ALU.add), reads=[bank], pwrites=[R1])
            R1.seal()
            S.fence()

    def ffn(layer, clo):
        coltiles = ctiles(clo, NC) if clo == 0 else btiles(clo, NC)
        isMoe = layer == 1
        ncols = NC - clo
        with ExitStack() as es_:
            h_h = es_.enter_context(sbt("f_h", [128, KC, NC], BF16))
            a_h = es_.enter_context(sbt("f_a", [128, 11, NC], BF16))
            wgu_h = es_.enter_context(sbt("f_wgu", [128, 2, 2, KC, 128], BF16))
            wd_h = es_.enter_context(sbt("f_wd", [128, 2, 11, 128], BF16))
            sd_h = es_.enter_context(sbt("f_sd", [128, 512], F32))
            sg_h = es_.enter_context(sbt("f_sg", [128, 2, 512], F32))
            aflat = a_h[:].rearrange("p j c -> p (j c)")
            hf_h = aflat[:, 0:8192].bitcast(F32).rearrange("p (k c) -> p k c", k=KC)
            sq_h = aflat[:, 8192:8192 + 4096].rearrange("p (k c) -> p k c", k=KC)
            hT = Tile("f_h", h_h)
            aT = Tile("f_a", a_h)
            wgu = [[Tile("f_wgu%d%d" % (b, j), wgu_h[:, b, j]) for j in range(2)] for b in range(2)]
            wd = [Tile("f_wd%d" % b, wd_h[:, b]) for b in range(2)]
            sq = Tile("f_sq", sq_h)
            sd = Tile("f_sd", sd_h)
            sg = [Tile("f_sg%d" % b, sg_h[:, b]) for b in range(2)]
            hf = Tile("f_hf", hf_h)
            S.tiles += [hT, aT, sq, sd, hf] + sg + wd + wgu[0] + wgu[1]
            if isMoe:
                gb_h = es_.enter_context(sbt("f_gb", [128, NC], BF16))
                wr_h = es_.enter_context(sbt("f_wr", [128, KC, 8], F32))
                gT_h = es_.enter_context(sbt("f_gT", [8, NC], F32))
                rt_h = es_.enter_context(sbt("f_rt", [128, 4, 64], F32))
                gb = Tile("f_gb", gb_h)
                wr = Tile("f_wr", wr_h)
                gT = Tile("f_gT", gT_h)
                rtT = [Tile("f_rt%d" % t, rt_h[:, t]) for t in range(4)]
                S.tiles += [gb, wr, gT] + rtT
            gcol = PV_NFD if not isMoe else PV_NFM
            if isMoe:
                S.dma("sp", wr.h[:], w_rt.h.rearrange("(k p) e -> p k e", p=128), writes=[wr])
            for (c0, n) in coltiles:
                if not isMoe:
                    norm_cols(lambda k: xT[:, :, c0:c0 + n] if k is None else xT[:, k, c0:c0 + n], n, gcol,
                              lambda k: hT.h[:, k, c0:c0 + n], hT, [R1], (sq, sd, PS[2]))
                else:
                    norm_cols(lambda k: xT[:, :, c0:c0 + n] if k is None else xT[:, k, c0:c0 + n], n, gcol,
                              lambda k: hf.h[:, k, 0:n], hf, [R1], (sq, sd, PS[2]))
                    hf.seal()
                    S.op("act", lambda e, c0=c0, n=n: e.activation(out=hT.h[:, :, c0:c0 + n], in_=hf.h[:, :, 0:n], func=AF.Copy),
                         reads=[hf], pwrites=[hT])
                    subs = ctiles(0, n, 128)

                    def mmr(e):
                        for t, (t0, tn) in enumerate(subs):
                            for k in range(KC):
                                i = e.matmul(PS[3].h[0:tn, t * 8:(t + 1) * 8], lhsT=hf.h[:, k, t0:t0 + tn], rhs=wr.h[:, k, :],
                                             start=(k == 0), stop=(k == KC - 1))
                        return i
                    S.op("pe", mmr, reads=[hf, wr], writes=[PS[3]])
                    for t, (t0, tn) in enumerate(subs):
                        S.op("dve", lambda e, t=t, tn=tn: e.tensor_copy(out=rtT[t].h[0:tn, 0:8], in_=PS[3].h[0:tn, t * 8:(t + 1) * 8]),
                             reads=[PS[3]], writes=[rtT[t]])
                    for t, (t0, tn) in enumerate(subs):
                        S.op("dve", lambda e, t=t, tn=tn: e.max(out=rtT[t].h[0:tn, 8:16], in_=rtT[t].h[0:tn, 0:8]), reads=[rtT[t]], writes=[rtT[t]])
                    for t, (t0, tn) in enumerate(subs):
                        S.op("dve", lambda e, t=t, tn=tn: e.tensor_scalar(out=rtT[t].h[0:tn, 16:24], in0=rtT[t].h[0:tn, 0:8], scalar1=rtT[t].h[0:tn, 9:10],
                                                                         scalar2=None, op0=ALU.is_ge), reads=[rtT[t]], writes=[rtT[t]])
                    for t, (t0, tn) in enumerate(subs):
                        S.op("dve", lambda e, t=t, tn=tn: e.tensor_scalar(out=rtT[t].h[0:tn, 24:25], in0=rtT[t].h[0:tn, 8:9], scalar1=-1.0, scalar2=None,
                                                                         op0=ALU.mult), reads=[rtT[t]], writes=[rtT[t]])
                    for t, (t0, tn) in enumerate(subs):
                        S.op("act", lambda e, t=t, tn=tn: e.activation(out=rtT[t].h[0:tn, 32:40], in_=rtT[t].h[0:tn, 0:8], func=AF.Exp,
                                                                      bias=rtT[t].h[0:tn, 24:25], scale=1.0), reads=[rtT[t]], writes=[rtT[t]])
                    for t, (t0, tn) in enumerate(subs):
                        S.op("dve", lambda e, t=t, tn=tn: e.tensor_tensor(out=rtT[t].h[0:tn, 32:40], in0=rtT[t].h[0:tn, 32:40], in1=rtT[t].h[0:tn, 16:24],
                                                                         op=ALU.mult), reads=[rtT[t]], writes=[rtT[t]])
                    for t, (t0, tn) in enumerate(subs):
                        S.op("dve", lambda e, t=t, tn=tn: e.tensor_reduce(out=rtT[t].h[0:tn, 25:26], in_=rtT[t].h[0:tn, 32:40], axis=AX.X, op=ALU.add),
                             reads=[rtT[t]], writes=[rtT[t]])
                    for t, (t0, tn) in enumerate(subs):
                        S.op("dve", lambda e, t=t, tn=tn: e.reciprocal(out=rtT[t].h[0:tn, 25:26], in_=rtT[t].h[0:tn, 25:26]), reads=[rtT[t]], writes=[rtT[t]])
                    for t, (t0, tn) in enumerate(subs):
                        S.op("dve", lambda e, t=t, tn=tn: e.tensor_scalar(out=rtT[t].h[0:tn, 40:48], in0=rtT[t].h[0:tn, 32:40], scalar1=rtT[t].h[0:tn, 25:26],
                                                                         scalar2=None, op0=ALU.mult), reads=[rtT[t]], writes=[rtT[t]])

                    def trg(e):
                        for t, (t0, tn) in enumerate(subs):
                            i = e.transpose(PS[4].h[0:8, t0:t0 + tn], rtT[t].h[0:tn, 40:48], ident[0:tn, 0:tn])
                        return i
                    S.op("pe", trg, reads=[rtT[t] for t in range(len(subs))] + [cst], writes=[PS[4]])
                    S.op("act", lambda e, c0=c0, n=n: e.activation(out=gT.h[0:8, c0:c0 + n], in_=PS[4].h[0:8, 0:n], func=AF.Copy),
                         reads=[PS[4]], pwrites=[gT])
            hT.seal()
            if isMoe:
                gT.seal()
            S.fence()

            nexp = 8 if isMoe else 2
            cw = {"gu": 0, "d": 0, "ps": 0}
            for ex in range(nexp):
                if isMoe:
                    for (c0, n) in coltiles:
                        S.op("pe", lambda e, c0=c0, n=n: e.matmul(PS[7].h[:, 0:n], lhsT=cst.h[0:8, C_SEL + ex * 128:C_SEL + (ex + 1) * 128],
                                                                  rhs=gT.h[0:8, c0:c0 + n], start=True, stop=True), reads=[gT, cst], writes=[PS[7]])
                        S.op("act", lambda e, c0=c0, n=n: e.activation(out=gb.h[:, c0:c0 + n], in_=PS[7].h[:, 0:n], func=AF.Copy), reads=[PS[7]], pwrites=[gb])
                    gb.seal()
                for j in range(11):
                    wb = wgu[cw["gu"] % 2]
                    cw["gu"] += 1
                    if isMoe:
                        srcg = w_gu_m.h[ex, :, j * 128:(j + 1) * 128]
                        srcu = w_gu_m.h[ex, :, 1408 + j * 128:1408 + (j + 1) * 128]
                    else:
                        jj = ex * 11 + j
                        srcg = w_gu_d.h[:, jj * 128:(jj + 1) * 128]
                        srcu = w_gu_d.h[:, 2816 + jj * 128:2816 + (jj + 1) * 128]
                    S.dma("pool", wb[0].h[:], srcg.rearrange("(k p) c -> p k c", p=128), writes=[wb[0]])
                    S.dma("pool", wb[1].h[:], srcu.rearrange("(k p) c -> p k c", p=128), writes=[wb[1]])
                    for (c0, n) in coltiles:
                        pg = PS[cw["ps"] % 2 * 2]
                        pu = PS[cw["ps"] % 2 * 2 + 1]
                        sgt = sg[cw["ps"] % 2]
                        cw["ps"] += 1

                        def mmg(e, which, bank, c0=c0, n=n, wb=wb):
                            for k in range(KC):
                                i = e.matmul(bank.h[:, 0:n], lhsT=wb[which].h[:, k, :], rhs=hT.h[:, k, c0:c0 + n], start=(k == 0), stop=(k == KC - 1))
                            return i
                        S.op("pe", lambda e, pg=pg: mmg(e, 0, pg), reads=[wb[0], hT], writes=[pg])
                        S.op("pe", lambda e, pu=pu: mmg(e, 1, pu), reads=[wb[1], hT], writes=[pu])
                        S.op("act", lambda e, pg=pg, sgt=sgt, n=n: e.activation(out=sgt.h[:, 0:n], in_=pg.h[:, 0:n], func=AF.Silu), reads=[pg], writes=[sgt])
                        if isMoe:
                            S.op("dve", lambda e, pu=pu, sgt=sgt, n=n: e.tensor_tensor(out=sgt.h[:, 0:n], in0=sgt.h[:, 0:n], in1=pu.h[:, 0:n], op=ALU.mult),
                                 reads=[pu, sgt], writes=[sgt])
                            S.op("dve", lambda e, sgt=sgt, c0=c0, n=n, j=j: e.tensor_tensor(out=aT.h[:, j, c0:c0 + n], in0=sgt.h[:, 0:n], in1=gb.h[:, c0:c0 + n], op=ALU.mult),
                                 reads=[sgt, gb], pwrites=[aT])
                        else:
                            S.op("dve", lambda e, pu=pu, sgt=sgt, c0=c0, n=n, j=j: e.tensor_tensor(out=aT.h[:, j, c0:c0 + n], in0=sgt.h[:, 0:n], in1=pu.h[:, 0:n], op=ALU.mult),
                                 reads=[pu, sgt], pwrites=[aT])
                aT.seal()
                for dc in range(KC):
                    wdt = wd[cw["d"] % 2]
                    cw["d"] += 1
                    if isMoe:
                        srcd = w_dn_m.h[ex, :, dc * 128:(dc + 1) * 128]
                    else:
                        srcd = w_dn_d.h[ex * 1408:(ex + 1) * 1408, dc * 128:(dc + 1) * 128]
                    S.dma("pool", wdt.h[:], srcd.rearrange("(j p) c -> p j c", p=128), writes=[wdt])
                    for (c0, n) in coltiles:
                        py = PS[4 + cw["ps"] % 2]
                        cw["ps"] += 1

                        def mmd2(e, py=py, wdt=wdt, c0=c0, n=n):
                            for j in range(11):
                                i = e.matmul(py.h[:, 0:n], lhsT=wdt.h[:, j, :], rhs=aT.h[:, j, c0:c0 + n], start=(j == 0), stop=(j == 10))
                            return i
                        S.op("pe", mmd2, reads=[wdt, aT], writes=[py])
                        S.op("dve", lambda e, py=py, dc=dc, c0=c0, n=n: e.tensor_tensor(out=xT[:, dc, c0:c0 + n], in0=xT[:, dc, c0:c0 + n], in1=py.h[:, 0:n], op=ALU.add),
                             reads=[py], pwrites=[R1])
                R1.seal()
            S.fence()

    try:
        def dump(name):
            if dbg and name in dbg:
                S.dma("sp", dbg_out[name].h.rearrange("(p k) a -> p k a", p=128), xT[:, :, :], reads=[R1])
                S.fence()

        stop_at('p0')
        attention_layer(0, hA, NKV, R1)
        stop_at('a0')
        out_proj(0, 0)
        stop_at('o0')
        dump("d_x1")
        ffn(0, 0)
        stop_at('f0')
        dump("d_x2")
        xTd = S.dram("xTd", [128, KC * NC], F32)
        with sbt("hB", [128, KC, NC], BF16) as hB_h:
            hB = Tile("hB", hB_h)
            S.tiles.append(hB)
            with sbt("n_sq", [128, KC, 512], BF16) as sq_h, sbt("n_sd", [128, 512], F32) as sd_h:
                sq = Tile("n_sq", sq_h)
                sd = Tile("n_sd", sd_h)
                S.tiles += [sq, sd]
                for (c0, n) in ctiles(0, NC):
                    norm_cols(lambda k: xT[:, :, c0:c0 + n] if k is None else xT[:, k, c0:c0 + n], n, PV_NMB,
                              lambda k: hB_h[:, k, c0:c0 + n], hB, [R1], (sq, sd, PS[2]))
                hB.seal()
                S.dma("sp", xTd.h, xT.rearrange("p k a -> p (k a)"), reads=[R1], writes=[xTd])
                S.fence()
            hB1 = R1.h[:, 0:KC * NC * 2].bitcast(BF16).rearrange("p (k a) -> p k a", k=KC)
            for k in range(KC):
                S.op("act" if k % 2 else "dve",
                     (lambda e, k=k: e.activation(out=hB1[:, k, :], in_=hB_h[:, k, :], func=AF.Copy)) if k % 2 else
                     (lambda e, k=k: e.tensor_copy(out=hB1[:, k, :], in_=hB_h[:, k, :])), reads=[hB], pwrites=[R1])
            R1.seal()
            S.fence()
        attention_layer(1, hB1, 0, R1)
        stop_at('a1')
        out_proj(1, 128)
        stop_at('o1')
        dump("d_x3")
        ffn(1, 128)
        stop_at('f1')
        with sbt("y_st", [128, 2, D], F32) as yst_h:
            yst = [Tile("y_st%d" % i, yst_h[:, i, :]) for i in range(2)]
            S.tiles += yst
            for ti, (c0, n) in enumerate(ctiles(128, NC, 128)):
                st = yst[ti % 2]
                for half in range(2):
                    bank = PS[(2 * ti + half) % 4]

                    def tr(e, half=half, bank=bank):
                        for kk in range(4):
                            k = half * 4 + kk
                            i = e.transpose(bank.h[0:n, kk * 128:(kk + 1) * 128], xT[:, k, c0:c0 + n], ident)
                        return i
                    S.op("pe", tr, reads=[R1, cst], writes=[bank])
                    S.op("act", lambda e, half=half, bank=bank: e.activation(out=st.h[0:n, half * 512:(half + 1) * 512], in_=bank.h[0:n, :], func=AF.Copy),
                         reads=[bank], pwrites=[st])
                st.seal()
                if c0 < NT:
                    S.dma("sp", y_p.h[c0 - 128:c0 - 128 + n, :], st.h[0:n, :], reads=[st], pwrites=[y_p], owner=st)
                else:
                    S.dma("sp", y_s.h[:, :], st.h[0:n, :], reads=[st], pwrites=[y_s], owner=st)
            S.fence()

    except _Stop:
        pass
    return nc, S


def _consts():
    c = np.zeros((128, C_N), np.float32)
    c[:, C_ID:C_ID + 128] = np.eye(128, dtype=np.float32)
    hd = np.arange(128) // 64
    same = (hd[:, None] == hd[None, :]).astype(np.float32)
    c[:, C_BD64:C_BD64 + 128] = same / 64.0
    c[:, C_BD1:C_BD1 + 128] = same
    i = np.arange(128)[:, None].astype(np.float32)
    j = np.arange(128)[None, :].astype(np.float32)
    c[:, C_DNEG:C_DNEG + 128] = -(j - i + 128.0)
    c[:, C_DNEG + 128:C_DNEG + 256] = -(j - i)
    c[:, C_MASK:C_MASK + 128] = np.where(i >= j, 0.0, NEG)
    c[:, C_MASK + 128:C_MASK + 256] = np.where(i <= j, 0.0, NEG)
    c[:, C_ONES:C_ONES + 128] = 1.0
    rho = np.arange(128, dtype=np.float32)
    for g in range(3):
        for h in range(16):
            c[:, C_AB + g * 16 + h] = -SLOPES[h] * GD[g] * (128.0 - rho)
    for hh, (ca_, cb_) in enumerate(((C_A0, C_B0), (C_A1, C_B1))):
        for m in range(64):
            c[hh * 64 + m, ca_ + m] = 1.0
        c[hh * 64:(hh + 1) * 64, cb_ + 64:cb_ + 128] = 1.0 / 64.0
    for e in range(8):
        c[e, C_SEL + e * 128:C_SEL + (e + 1) * 128] = 1.0
    return c


def _valid(q):
    s1 = 2048 * q - 128
    v = np.zeros((128, 112), np.float32)
    i = np.arange(128)
    col = 0
    for g, d in enumerate(GD):
        U = NT // d
        nb = (U + 127) // 128 + 1
        for r in range(d):
            for bb in range(nb):
                u = 128 * (bb - 1) + i
                tok = s1 + u * d + r
                v[:, col] = (tok >= 0).astype(np.float32)
                col += 1
    for bb in range(18):
        tok = s1 + 128 * bb + i
        v[:, col] = (tok >= 0).astype(np.float32)
        col += 1
    return v


def _tile2(vec64):
    return np.concatenate([vec64, vec64]).astype(np.float32)


_CACHE = {}


def kernel(x_prompt, x_sample, cache_a_w128, cache_a_w512, cache_a_w2048, cache_b,
           norm_mix_a, w_in_a, q_gain_a, k_gain_a, w_out_a, norm_ffn_dense, w_gu_dense, w_down_dense,
           norm_mix_b, w_in_b, q_gain_b, k_gain_b, sink_b, w_out_b, norm_ffn_moe, w_router,
           w_gu_moe, w_down_moe, _dbg=None, _stop=None, _cores=None):
    f = lambda a: np.ascontiguousarray(np.asarray(a, dtype=np.float32))
    x_prompt, x_sample = f(x_prompt), f(x_sample)
    key = (tuple(sorted(_dbg.items())) if _dbg else None, _stop)
    if key not in _CACHE:
        _CACHE[key] = build_program(_dbg, _stop)
    nc, S = _CACHE[key]

    pvec = np.zeros((128, PV_N), np.float32)
    for col, v in ((PV_NMA, norm_mix_a), (PV_NFD, norm_ffn_dense), (PV_NMB, norm_mix_b), (PV_NFM, norm_ffn_moe)):
        pvec[:, col:col + 8] = f(v)[0].reshape(8, 128).T
    for g in range(3):
        pvec[:, PV_GQA + g] = _tile2(f(q_gain_a)[0, g])
        pvec[:, PV_GKA + g] = _tile2(f(k_gain_a)[0, g])
    pvec[:, PV_GQB] = _tile2(f(q_gain_b)[0])
    pvec[:, PV_GKB] = _tile2(f(k_gain_b)[0])
    pvec[:, PV_SINK:PV_SINK + 16] = f(sink_b)[0][None, :]
    cst = _consts()
    caches = [f(cache_a_w128)[0], f(cache_a_w512)[0], f(cache_a_w2048)[0]]
    cbf_ = f(cache_b)[0]
    shared = {
        "w_in_a": f(w_in_a)[0], "w_out_a": f(w_out_a)[0], "w_gu_dense": f(w_gu_dense)[0], "w_down_dense": f(w_down_dense)[0],
        "w_in_b": f(w_in_b)[0], "w_out_b": f(w_out_b)[0], "w_router": f(w_router)[0], "w_gu_moe": f(w_gu_moe)[0],
        "w_down_moe": f(w_down_moe)[0], "pvec": pvec, "cst": cst,
    }
    in_maps = []
    for c in range(8):
        b, q = c // 4, c % 4
        xin = np.zeros((NA, D), np.float32)
        t0 = 2048 * q - 128 - NKV
        lo = max(0, t0)
        xin[lo - t0:NKV + NT] = x_prompt[b, lo:t0 + NKV + NT]
        xin[NKV + NT:] = x_sample[16 * c:16 * c + 16, 0]
        m = dict(shared)
        m["xin"] = xin
        for g, d in enumerate(GD):
            m["ca%d" % g] = np.ascontiguousarray(caches[g][16 * c:16 * c + 16, 0::d].reshape(NS, 128, 2048))
        m["cb"] = np.ascontiguousarray(cbf_[16 * c:16 * c + 16].reshape(NS, 128, 256))
        m["vld"] = _valid(q)
        in_maps.append(m)
    if _cores is not None:
        res = run_bass_kernel_spmd(nc, [in_maps[c] for c in _cores], core_ids=list(range(len(_cores))))
        return res.results
    res = run_bass_kernel_spmd(nc, in_maps, core_ids=list(range(8)))
    R = res.results
    y_prompt = np.stack([np.concatenate([R[4 * b + q]["y_p"] for q in range(4)], axis=0) for b in range(2)], axis=0)
    y_sample = np.concatenate([R[c]["y_s"] for c in range(8)], axis=0).reshape(128, 1, D)
    outs = [y_prompt.astype(np.float32), y_sample.astype(np.float32)]
    for g, d in enumerate(GD):
        ap_ = np.stack([R[3]["kvp%d" % g], R[7]["kvp%d" % g]], axis=0).reshape(1, 2, 128 * d, 2, 16, 64)
        as_ = np.concatenate([R[c]["kvs%d" % g] for c in range(8)], axis=0).reshape(1, 128, 1, 2, 16, 64)
        outs += [ap_.astype(np.float32), as_.astype(np.float32)]
    b_p = np.stack([R[3]["bp"], R[7]["bp"]], axis=0).reshape(1, 2, 128, 2, 2, 64)
    b_s = np.concatenate([R[c]["bs"] for c in range(8)], axis=0).reshape(1, 128, 1, 2, 2, 64)
    outs += [b_p.astype(np.float32), b_s.astype(np.float32)]
    if _dbg:
        return tuple(outs), R
    return tuple(outs)
```
